# Optimizing a Trainium2 kernel written in Bass

```python
import math
import jax, jax.numpy as jnp
from jax import lax
import numpy as np

D_MODEL = 2048
BATCH = 2
SEQ = 8192
DEPTH = 1

GRID_W = 64
CTX_LEN = 256
EPS = 1e-6

SSM_HEAD_DIM = 64
SSM_D_INNER = 2 * D_MODEL
SSM_HEADS = SSM_D_INNER // SSM_HEAD_DIM
SSM_GROUPS = 8
SSM_STATE = 128
SSM_CONV = 7
SSM_CHUNK = 128
XBC_DIM = SSM_D_INNER + 2 * SSM_GROUPS * SSM_STATE

CONV_DIM = D_MODEL
CONV_KERNEL = 31

OFF_DT = XBC_DIM
OFF_Z = OFF_DT + SSM_HEADS
OFF_GLU = OFF_Z + SSM_D_INNER
OFF_GATE = OFF_GLU + 2 * CONV_DIM
IN_PROJ_DIM = OFF_GATE + 2 * D_MODEL

MOE_GROUPS = 8
EXPERTS_PER_GROUP = 8
N_EXPERTS = MOE_GROUPS * EXPERTS_PER_GROUP
TOP_K = 2
D_FF_EXPERT = D_MODEL // 2
MOE_BLOCK = 256

kernel_name = "hybrid_ssd_conformer_hmoe_dit"


def rms_norm(x, w):
    xf = x.astype(jnp.float32)
    y = xf * lax.rsqrt(jnp.mean(xf * xf, axis=-1, keepdims=True) + EPS)
    return (y * w.astype(jnp.float32)).astype(x.dtype)


def layer_norm(x, w, b):
    xf = x.astype(jnp.float32)
    mu = jnp.mean(xf, axis=-1, keepdims=True)
    var = jnp.mean(jnp.square(xf - mu), axis=-1, keepdims=True)
    y = (xf - mu) * lax.rsqrt(var + EPS)
    return (y * w.astype(jnp.float32) + b.astype(jnp.float32)).astype(x.dtype)


def modulate(h, shift, scale):
    return h * (1 + scale) + shift


def conv_1d_centred(x, w, b):
    k = w.shape[0]
    y = lax.conv_general_dilated(x, w[:, None, :].astype(x.dtype), window_strides=(1,),
                                 padding=[(k // 2, k // 2)],
                                 dimension_numbers=("NWC", "WIO", "NWC"),
                                 feature_group_count=x.shape[-1])
    return y + b.astype(x.dtype)


def conv_grid_columns(x, w, b):
    bsz, n_tok, ch = x.shape
    rows = n_tok // GRID_W
    g = x.reshape(bsz, rows, GRID_W, ch)
    k = w.shape[0]
    y = lax.conv_general_dilated(g, w[:, None, None, :].astype(x.dtype), (1, 1),
                                 [(k // 2, k // 2), (0, 0)],
                                 dimension_numbers=("NHWC", "HWIO", "NHWC"),
                                 feature_group_count=ch)
    return (y + b.astype(x.dtype)).reshape(bsz, n_tok, ch)


def ssd_chunked(xs, dt, a, bm, cm, h0, with_output):
    bsz, n_tok, n_heads, p = xs.shape
    g, n = bm.shape[-2:]
    r = n_heads // g
    nc = n_tok // SSM_CHUNK
    f32 = jnp.float32
    x = xs.astype(f32).reshape(bsz, nc, SSM_CHUNK, g, r, p)
    dtc = dt.reshape(bsz, nc, SSM_CHUNK, g, r)
    bc = bm.astype(f32).reshape(bsz, nc, SSM_CHUNK, g, n)
    xdt = x * dtc[..., None]
    cum = jnp.cumsum(dtc * a.reshape(g, r), axis=2)
    to_end = jnp.exp(cum[:, :, -1:] - cum)
    states = jnp.einsum("bcsgn,bcsgrp->bcgrpn", bc, xdt * to_end[..., None])
    chunk_decay = jnp.exp(cum[:, :, -1])

    def step(h, inp):
        s, d = inp
        return h * d[..., None, None] + s, h

    h_last, h_in = lax.scan(step, h0.reshape(bsz, g, r, p, n),
                            (jnp.moveaxis(states, 1, 0), jnp.moveaxis(chunk_decay, 1, 0)))
    h_last = h_last.reshape(bsz, n_heads, p, n)
    if not with_output:
        return None, h_last
    h_in = jnp.moveaxis(h_in, 0, 1)
    cc = cm.astype(f32).reshape(bsz, nc, SSM_CHUNK, g, n)
    idx = jnp.arange(SSM_CHUNK)
    lower = (idx[:, None] >= idx[None, :])[:, :, None, None]
    seg = cum[:, :, :, None] - cum[:, :, None, :]
    decay = jnp.exp(jnp.where(lower, seg, -jnp.inf))
    scores = jnp.einsum("bclgn,bcsgn->bclsg", cc, bc)
    y_diag = jnp.einsum("bclsgr,bcsgrp->bclgrp", scores[..., None] * decay, xdt)
    y_off = jnp.einsum("bclgn,bcgrpn->bclgrp", cc, h_in) * jnp.exp(cum)[..., None]
    return (y_diag + y_off).reshape(bsz, n_tok, n_heads, p), h_last


def ssm_inputs(proj, conv_w, conv_b):
    bsz, n_tok, _ = proj.shape
    gn = SSM_GROUPS * SSM_STATE
    xbc = jax.nn.silu(conv_1d_centred(proj[..., :XBC_DIM], conv_w, conv_b))
    xs = xbc[..., :SSM_D_INNER].reshape(bsz, n_tok, SSM_HEADS, SSM_HEAD_DIM)
    bm = xbc[..., SSM_D_INNER:SSM_D_INNER + gn].reshape(bsz, n_tok, SSM_GROUPS, SSM_STATE)
    cm = xbc[..., SSM_D_INNER + gn:XBC_DIM].reshape(bsz, n_tok, SSM_GROUPS, SSM_STATE)
    dt_raw = proj[..., OFF_DT:OFF_Z].astype(jnp.float32)
    return xs, bm, cm, dt_raw


def bidirectional_ssd(lat, ctx, dt_bias, a_log, d_skip, ctx_out):
    xs_l, b_l, c_l, dtr_l = lat
    xs_c, b_c, c_c, dtr_c = ctx
    bsz = xs_l.shape[0]
    h_zero = jnp.zeros((bsz, SSM_HEADS, SSM_HEAD_DIM, SSM_STATE), jnp.float32)
    ys_lat, ys_ctx = [], []
    for k in range(2):
        rev = (lambda t: jnp.flip(t, axis=1)) if k == 1 else (lambda t: t)
        a = -jnp.exp(a_log[k].astype(jnp.float32))
        bias = dt_bias[k].astype(jnp.float32)
        d = d_skip[k].astype(jnp.float32)[:, None]
        dt_c = jax.nn.softplus(dtr_c + bias)
        dt_l = jax.nn.softplus(dtr_l + bias)
        yc, hc = ssd_chunked(rev(xs_c), rev(dt_c), a, rev(b_c), rev(c_c), h_zero, ctx_out)
        yl, _ = ssd_chunked(rev(xs_l), rev(dt_l), a, rev(b_l), rev(c_l), hc, True)
        ys_lat.append(rev(yl) + d * xs_l.astype(jnp.float32))
        if ctx_out:
            ys_ctx.append(rev(yc) + d * xs_c.astype(jnp.float32))
    y_ctx = ys_ctx[0] + ys_ctx[1] if ctx_out else None
    return ys_lat[0] + ys_lat[1], y_ctx


def branch_merge(proj, y_heads, conv_fn, ssm_norm_w, ssm_out_w, cf_dw_w, cf_dw_b,
                 cf_ln_w, cf_ln_b, cf_out_w, cf_out_b, w_o):
    bsz, n_tok, _ = proj.shape
    z = proj[..., OFF_Z:OFF_GLU]
    y = y_heads.reshape(bsz, n_tok, SSM_D_INNER).astype(proj.dtype)
    y_ssd = rms_norm(y * jax.nn.silu(z), ssm_norm_w) @ ssm_out_w
    glu = proj[..., OFF_GLU:OFF_GATE]
    u = glu[..., :CONV_DIM] * jax.nn.sigmoid(glu[..., CONV_DIM:])
    u = jax.nn.silu(layer_norm(conv_fn(u, cf_dw_w, cf_dw_b), cf_ln_w, cf_ln_b))
    y_cf = u @ cf_out_w + cf_out_b
    gate = jax.nn.sigmoid(proj[..., OFF_GATE:])
    merged = gate[..., :D_MODEL] * y_ssd + gate[..., D_MODEL:] * y_cf
    return merged @ w_o


def hierarchical_moe(h, rg_w, rg_b, re_w, re_b, w_gate, w_up, w_down):
    n_tok, d = h.shape
    f32 = jnp.float32
    g_prob = jax.nn.softmax((h @ rg_w + rg_b).astype(f32), axis=-1)
    g_p, g_idx = lax.top_k(g_prob, 1)
    e_logits = (h @ re_w + re_b).astype(f32).reshape(n_tok, MOE_GROUPS, EXPERTS_PER_GROUP)
    sel = jnp.broadcast_to(g_idx[:, :, None], (n_tok, 1, EXPERTS_PER_GROUP))
    e_in = jnp.take_along_axis(e_logits, sel, axis=1)[:, 0]
    e_l, e_idx = lax.top_k(e_in, TOP_K)
    weight = jax.nn.softmax(e_l, axis=-1) * g_p
    expert = (g_idx * EXPERTS_PER_GROUP + e_idx).reshape(-1)
    n_assign = n_tok * TOP_K
    order = jnp.argsort(expert)
    e_sorted = expert[order]
    tok_sorted = (order // TOP_K).astype(jnp.int32)
    w_sorted = weight.reshape(-1)[order]
    counts = jnp.bincount(expert, length=N_EXPERTS)
    padded = (counts + MOE_BLOCK - 1) // MOE_BLOCK * MOE_BLOCK
    start = jnp.cumsum(counts) - counts
    pad_end = jnp.cumsum(padded)
    pad_start = pad_end - padded
    dest = pad_start[e_sorted] + jnp.arange(n_assign) - start[e_sorted]
    n_blocks = -(-n_assign // MOE_BLOCK) + N_EXPERTS
    buf_tok = jnp.zeros((n_blocks * MOE_BLOCK,), jnp.int32).at[dest].set(tok_sorted)
    block_expert = jnp.minimum(
        jnp.searchsorted(pad_end, jnp.arange(n_blocks) * MOE_BLOCK, side="right"), N_EXPERTS - 1)
    xb = h[buf_tok].reshape(n_blocks, MOE_BLOCK, d)

    def expert_block(args):
        xe, e = args
        hid = jax.nn.silu(xe @ w_gate[e]) * (xe @ w_up[e])
        return hid @ w_down[e]

    yb = lax.map(expert_block, (xb, block_expert)).reshape(n_blocks * MOE_BLOCK, d)
    contrib = yb[dest].astype(f32) * w_sorted[:, None]
    return jnp.zeros((n_tok, d), f32).at[tok_sorted].add(contrib).astype(h.dtype)


def _normal(k, shape, scale):
    return jax.random.normal(k, shape, jnp.float32) * scale


def setup_inputs(seed: int = 0) -> dict:
    key = jax.random.key(seed)
    ks = jax.random.split(key, 40)
    L, D = DEPTH, D_MODEL
    dt0 = jnp.exp(jax.random.uniform(ks[10], (L, 2, SSM_HEADS), jnp.float32,
                                     math.log(1e-3), math.log(1e-1)))
    return {
        "x": _normal(ks[0], (BATCH, SEQ, D), 1.0),
        "c": _normal(ks[1], (BATCH, D), 1.0),
        "ctx": _normal(ks[2], (BATCH, CTX_LEN, D), 1.0),
        "c_ctx": _normal(ks[3], (D,), 1.0),
        "ada_w": _normal(ks[4], (L, D, 6 * D), 0.5 * D ** -0.5),
        "ada_b": _normal(ks[5], (L, 6 * D), 0.02),
        "norm1_w": 1.0 + _normal(ks[6], (L, D), 0.02),
        "w_in": _normal(ks[7], (L, D, IN_PROJ_DIM), D ** -0.5),
        "ssm_conv_w": _normal(ks[8], (L, SSM_CONV, XBC_DIM), SSM_CONV ** -0.5),
        "ssm_conv_b": _normal(ks[9], (L, XBC_DIM), 0.02),
        "dt_bias": dt0 + jnp.log(-jnp.expm1(-dt0)),
        "a_log": jnp.log(jax.random.uniform(ks[11], (L, 2, SSM_HEADS), jnp.float32, 1.0, 16.0)),
        "d_skip": 1.0 + _normal(ks[12], (L, 2, SSM_HEADS), 0.02),
        "ssm_norm_w": 1.0 + _normal(ks[13], (L, SSM_D_INNER), 0.02),
        "ssm_out_w": _normal(ks[14], (L, SSM_D_INNER, D), SSM_D_INNER ** -0.5),
        "cf_dw_w": _normal(ks[15], (L, CONV_KERNEL, CONV_DIM), CONV_KERNEL ** -0.5),
        "cf_dw_b": _normal(ks[16], (L, CONV_DIM), 0.02),
        "cf_ln_w": 1.0 + _normal(ks[17], (L, CONV_DIM), 0.02),
        "cf_ln_b": _normal(ks[18], (L, CONV_DIM), 0.02),
        "cf_out_w": _normal(ks[19], (L, CONV_DIM, D), CONV_DIM ** -0.5),
        "cf_out_b": _normal(ks[20], (L, D), 0.02),
        "w_o": _normal(ks[21], (L, D, D), D ** -0.5),
        "norm2_w": 1.0 + _normal(ks[22], (L, D), 0.02),
        "router_group_w": _normal(ks[23], (L, D, MOE_GROUPS), D ** -0.5),
        "router_group_b": _normal(ks[24], (L, MOE_GROUPS), 0.01),
        "router_expert_w": _normal(ks[25], (L, D, N_EXPERTS), D ** -0.5),
        "router_expert_b": _normal(ks[26], (L, N_EXPERTS), 0.01),
        "expert_w_gate": _normal(ks[27], (L, N_EXPERTS, D, D_FF_EXPERT), D ** -0.5),
        "expert_w_up": _normal(ks[28], (L, N_EXPERTS, D, D_FF_EXPERT), D ** -0.5),
        "expert_w_down": _normal(ks[29], (L, N_EXPERTS, D_FF_EXPERT, D), D_FF_EXPERT ** -0.5),
        "final_norm_w": 1.0 + _normal(ks[30], (D,), 0.02),
    }


def reference(x, c, ctx, c_ctx, ada_w, ada_b, norm1_w, w_in, ssm_conv_w, ssm_conv_b,
              dt_bias, a_log, d_skip, ssm_norm_w, ssm_out_w, cf_dw_w, cf_dw_b, cf_ln_w,
              cf_ln_b, cf_out_w, cf_out_b, w_o, norm2_w, router_group_w, router_group_b,
              router_expert_w, router_expert_b, expert_w_gate, expert_w_up, expert_w_down,
              final_norm_w):
    bsz, n_tok, d = x.shape
    ctx_h = ctx
    for i in range(DEPTH):
        last = i == DEPTH - 1
        mod = jax.nn.silu(c) @ ada_w[i] + ada_b[i]
        sh1, sc1, g1, sh2, sc2, g2 = jnp.split(mod[:, None, :], 6, axis=-1)
        mod_c = jax.nn.silu(c_ctx) @ ada_w[i] + ada_b[i]
        csh1, csc1, cg1, csh2, csc2, cg2 = jnp.split(mod_c, 6, axis=-1)

        h = modulate(rms_norm(x, norm1_w[i]), sh1, sc1)
        hc = modulate(rms_norm(ctx_h, norm1_w[i]), csh1, csc1)
        proj = h @ w_in[i]
        proj_c = hc @ (w_in[i][:, :OFF_Z] if last else w_in[i])
        y_lat, y_ctx = bidirectional_ssd(ssm_inputs(proj, ssm_conv_w[i], ssm_conv_b[i]),
                                         ssm_inputs(proj_c, ssm_conv_w[i], ssm_conv_b[i]),
                                         dt_bias[i], a_log[i], d_skip[i], not last)
        x = x + g1 * branch_merge(proj, y_lat, conv_grid_columns, ssm_norm_w[i], ssm_out_w[i],
                                  cf_dw_w[i], cf_dw_b[i], cf_ln_w[i], cf_ln_b[i],
                                  cf_out_w[i], cf_out_b[i], w_o[i])
        if not last:
            ctx_h = ctx_h + cg1 * branch_merge(proj_c, y_ctx, conv_1d_centred, ssm_norm_w[i],
                                               ssm_out_w[i], cf_dw_w[i], cf_dw_b[i], cf_ln_w[i],
                                               cf_ln_b[i], cf_out_w[i], cf_out_b[i], w_o[i])

        h2 = modulate(rms_norm(x, norm2_w[i]), sh2, sc2).reshape(-1, d)
        if last:
            out = hierarchical_moe(h2, router_group_w[i], router_group_b[i], router_expert_w[i],
                                   router_expert_b[i], expert_w_gate[i], expert_w_up[i],
                                   expert_w_down[i])
            x = x + g2 * out.reshape(x.shape)
        else:
            hc2 = modulate(rms_norm(ctx_h, norm2_w[i]), csh2, csc2).reshape(-1, d)
            out = hierarchical_moe(jnp.concatenate([h2, hc2], axis=0), router_group_w[i],
                                   router_group_b[i], router_expert_w[i], router_expert_b[i],
                                   expert_w_gate[i], expert_w_up[i], expert_w_down[i])
            n_lat = bsz * n_tok
            x = x + g2 * out[:n_lat].reshape(x.shape)
            ctx_h = ctx_h + cg2 * out[n_lat:].reshape(ctx_h.shape)
    return rms_norm(x, final_norm_w)
```

```python
import numpy as np
import ml_dtypes
from contextlib import ExitStack
import concourse.bass as bass
import concourse.mybir as mybir
from concourse.bass_utils import run_bass_kernel_spmd

F32 = mybir.dt.float32
BF16 = mybir.dt.bfloat16
AF = mybir.ActivationFunctionType
ALU = mybir.AluOpType
AX = mybir.AxisListType

D = 2048
DI = 4096
NH = 64
NG = 8
XBC = 6144
OFF_DT = 6144
OFF_Z = 6208
OFF_GLU = 10304
OFF_GATE = 14400
NPROJ = 18496
CTX = 256
GRID_W = 64
DFF = 1024
EL = 16
EPS = 1e-6
NEG = -30000.0


class Dep:
    __slots__ = ("w", "r")

    def __init__(self):
        self.w = None
        self.r = {}


class T:
    def __init__(self, t):
        self.t = t
        self.d = Dep()


class Q:
    def __init__(self, kb, name, eng, is_dma, stream):
        self.kb, self.name, self.eng, self.is_dma, self.stream = kb, name, eng, is_dma, stream
        self.n = 0
        self.sem = None
        self.last = {}
        if is_dma:
            self.sems = [kb.newsem(f"{name}_d{i}") for i in range(10)]
            self.cnt = [0] * len(self.sems)
            self.i = 0

    def wait(self, ev):
        sem, val = ev
        seen = self.stream
        if seen.get(id(sem), 0) >= val:
            return
        self.eng.wait_ge(sem, val)
        seen[id(sem)] = val

    def signal(self, inst):
        if self.is_dma:
            j = self.i % len(self.sems)
            self.i += 1
            self.cnt[j] += 16
            inst.then_inc(self.sems[j], 16)
            ev = (self.sems[j], self.cnt[j])
        else:
            if self.sem is None or self.n >= 20000:
                self.sem = self.kb.newsem(f"{self.name}_e{len(self.kb.sems)}")
                self.n = 0
            self.n += 1
            inst.then_inc(self.sem, 1)
            ev = (self.sem, self.n)
        self.last[id(ev[0])] = ev
        return ev

    def pre_dma(self):
        j = self.i % len(self.sems)
        if self.cnt[j] > 0:
            self.wait((self.sems[j], self.cnt[j]))


class KB:
    def __init__(self, nc, es):
        self.nc, self.es = nc, es
        self.sems = []
        s_pool, s_act, s_dve, s_pe, s_sp = {}, {}, {}, {}, {}
        self.pe = Q(self, "pe", nc.tensor, False, s_pe)
        self.act = Q(self, "act", nc.scalar, False, s_act)
        self.dve = Q(self, "dve", nc.vector, False, s_dve)
        self.pool = Q(self, "pool", nc.gpsimd, False, s_pool)
        self.ld = Q(self, "ld", nc.sync, True, s_sp)
        self.st = Q(self, "st", nc.gpsimd, True, s_pool)
        self.qs = [self.pe, self.act, self.dve, self.pool, self.ld, self.st]
        self.uid = 0

    def newsem(self, name):
        s = self.es.enter_context(self.nc.semaphore(name))
        self.sems.append(s)
        return s

    def sb(self, shape, dt, es=None):
        self.uid += 1
        return T((es or self.es).enter_context(self.nc.sbuf_tensor(f"sb{self.uid}", list(shape), dt)))

    def ps(self, shape, dt, es=None):
        self.uid += 1
        return T((es or self.es).enter_context(self.nc.psum_tensor(f"ps{self.uid}", list(shape), dt)))

    def op(self, q, fn, R=(), W=(), sig=True):
        deps = {}

        def add(ev):
            if ev is not None:
                k = id(ev[0])
                if k not in deps or deps[k][1] < ev[1]:
                    deps[k] = ev
        for x in R:
            add(x.d.w)
        for x in W:
            add(x.d.w)
            for ev in x.d.r.values():
                add(ev)
        if q.is_dma:
            q.pre_dma()
        for ev in deps.values():
            if q is self.pe and ev[0] is self.pe.sem:
                continue
            q.wait(ev)
        inst = fn(q.eng)
        if not sig:
            return None
        ev = q.signal(inst)
        for x in R:
            x.d.r[id(ev[0])] = ev
        for x in W:
            x.d.w = ev
            x.d.r = {}
        return ev

    def barrier(self):
        evs = []
        for q in self.qs:
            evs.extend(q.last.values())
        for q in self.qs:
            for ev in evs:
                q.wait(ev)


def build(SEQ, SBW, MB, debug=False):
    nc = bass.Bass("TRN2", target_bir_lowering=False)
    NT = SEQ // 128
    TALL = SEQ + CTX
    PAD = 15 * GRID_W

    def din(name, shape, dt=F32):
        return nc.dram_tensor(name, list(shape), dt, kind="ExternalInput").ap()

    def dscr(name, shape, dt):
        return nc.dram_tensor(name, list(shape), dt, kind="Internal").ap()

    x_in = din("x", [SEQ, D])
    ctx_in = din("ctx", [CTX, D])
    cvec_in = din("cvec", [128, 16, 2])
    ada_w = din("ada_w", [D, 6 * D])
    ada_b = din("ada_b", [2, 6 * D])
    n1w_in = din("n1w", [128, 16])
    n2w_in = din("n2w", [128, 16])
    fnw_in = din("fnw", [128, D])
    w_in = din("w_in", [D, NPROJ])
    cw_in = din("cw", [128, 48, 7])
    cb_in = din("cb", [128, 48])
    dtb_in = din("dtb", [128, 2, 64])
    alog_in = din("alog", [128, 2, 64])
    dsk_in = din("dsk", [128, 2, 64])
    snw_in = din("snw", [128, 32])
    wout_in = din("ssm_out_w", [DI, D])
    cfw_in = din("cfw", [128, 16, 31])
    cfb_in = din("cfb", [128, 16])
    lnw_in = din("lnw", [128, 16])
    lnb_in = din("lnb", [128, 16])
    wcf_in = din("cf_out_w", [D, D])
    cfob_in = din("cfob", [128, 16])
    wo_in = din("w_o", [D, D])
    rw_in = din("rw", [128, 16, 72])
    rb_in = din("rb", [128, 72])
    gsel_in = din("gsel", [128, 2, 8])
    eg_in = din("eg", [EL, D, DFF])
    eu_in = din("eu", [EL, D, DFF])
    ed_in = din("ed", [EL, DFF, D])
    ident_in = din("ident", [128, 128])
    tri_in = din("tri", [128, 2, 128])
    nmask_in = din("nmask", [128, 2, 128])

    out_d = nc.dram_tensor("out", [SEQ, D], F32, kind="ExternalOutput").ap()
    own_d = nc.dram_tensor("own", [128, NT], F32, kind="ExternalOutput").ap()

    XBCs = dscr("XBCs", [XBC, TALL], BF16)
    DTs = dscr("DTs", [TALL, 64], F32)
    ZS = dscr("ZS", [SEQ, DI], BF16)
    U = dscr("U", [D, SEQ], F32)
    GT = dscr("GT", [DI, SEQ], BF16)
    Y = dscr("Y", [SEQ, DI], F32)
    V = dscr("V", [D, SEQ], F32)
    X2 = dscr("X2", [SEQ, D], F32)
    H2T = dscr("H2T", [D, SEQ], BF16)
    MODD = dscr("MODD", [2, 6 * D], F32)
    WOUTB = dscr("WOUTB", [16, 128, 32, 128], BF16)
    WCFB = dscr("WCFB", [16, 128, 16, 128], BF16)
    WOB = dscr("WOB", [4, 128, 16, 512], BF16)
    EXG = dscr("EXG", [EL, 4, 128, 16, 256], BF16)
    EXU = dscr("EXU", [EL, 4, 128, 16, 256], BF16)
    EXD = dscr("EXD", [EL, 4, 128, 8, 512], BF16)

    with ExitStack() as es:
        kb = KB(nc, es)
        pe, act, dve, pool, ld, st = kb.pe, kb.act, kb.dve, kb.pool, kb.ld, kb.st
        op = kb.op

        def load(dst, dst_ap, src_ap):
            return op(ld, lambda e: e.dma_start(out=dst_ap, in_=src_ap), W=[dst])

        def store(src, dst_ap, src_ap):
            return op(st, lambda e: e.dma_start(out=dst_ap, in_=src_ap), R=[src])

        ident = kb.sb([128, 128], F32)
        identb = kb.sb([128, 128], BF16)
        ones = kb.sb([128, 128], F32)
        tri = kb.sb([128, 2, 128], F32)
        nmask = kb.sb([128, 2, 128], F32)
        modc = kb.sb([128, 96, 2], F32)
        A1 = kb.sb([128, 16], F32)
        Ac1 = kb.sb([128, 16], F32)
        A2 = kb.sb([128, 16], F32)
        g1B = kb.sb([128, D], F32)
        g2B = kb.sb([128, D], F32)
        coef = kb.sb([128, NT, EL], F32)
        own = kb.sb([128, NT], F32)
        load(ident, ident.t[:], ident_in[:, :])
        load(tri, tri.t[:], tri_in[:, :, :])
        load(nmask, nmask.t[:], nmask_in[:, :, :])
        op(dve, lambda e: e.tensor_copy(out=identb.t[:], in_=ident.t[:]), R=[ident], W=[identb])
        op(dve, lambda e: e.memset(ones.t[:], 1.0), W=[ones])

        psA = [kb.ps([128, 512], F32) for _ in range(3)]
        psX = kb.ps([128, 512], F32)
        psY = kb.ps([128, 512], F32)
        psT = [kb.ps([128, 1024], BF16) for _ in range(2)]
        psS = kb.ps([128, 512], F32)
        rot = {"a": 0, "t": 0}

        def nextA():
            rot["a"] += 1
            return psA[rot["a"] % len(psA)]

        def nextT():
            rot["t"] += 1
            return psT[rot["t"] % 2]

        def rms_rstd(es_l, xt, npart, ncol, junk):
            ss = kb.sb([128, 1], F32, es_l)
            rs = kb.sb([128, 1], F32, es_l)
            op(act, lambda e: e.activation(out=junk.t[0:npart, 0:ncol], in_=xt.t[0:npart, 0:ncol], func=AF.Square,
                                           accum_out=ss.t[0:npart, :]), R=[xt], W=[junk, ss])
            op(act, lambda e: e.activation(out=ss.t[0:npart, :], in_=ss.t[0:npart, :], func=AF.Sqrt,
                                           scale=1.0 / ncol, bias=epsc.t[0:npart, :]), R=[ss, epsc], W=[ss])
            op(dve, lambda e: e.reciprocal(out=rs.t[0:npart, :], in_=ss.t[0:npart, :]), R=[ss], W=[rs])
            return rs

        epsc = kb.sb([128, 1], F32)
        op(dve, lambda e: e.memset(epsc.t[:], EPS), W=[epsc])
        onec = kb.sb([128, 1], F32)
        op(dve, lambda e: e.memset(onec.t[:], 1.0), W=[onec])

        with ExitStack() as e0:
            cv = kb.sb([128, 16, 2], F32, e0)
            sc = kb.sb([128, 16, 2], F32, e0)
            modrow = kb.sb([2, 6 * D], F32, e0)
            adab = kb.sb([2, 6 * D], F32, e0)
            wts = [kb.sb([128, 16, 256], F32, e0) for _ in range(2)]
            load(cv, cv.t[:], cvec_in[:, :, :])
            load(adab, adab.t[:], ada_b[:, :])
            op(act, lambda e: e.activation(out=sc.t[:], in_=cv.t[:], func=AF.Silu), R=[cv], W=[sc])
            for gi in range(6 * D // 256):
                wt = wts[gi % 2]
                load(wt, wt.t[:], ada_w[:, gi * 256:(gi + 1) * 256].rearrange("(k p) c -> p k c", p=128))
                pt = nextA()
                for k in range(16):
                    op(pe, lambda e, k=k: e.matmul(pt.t[0:2, 0:256], lhsT=sc.t[:, k, :], rhs=wt.t[:, k, :],
                                                   start=(k == 0), stop=(k == 15)),
                       R=[sc, wt], W=[pt], sig=(k == 15))
                op(dve, lambda e: e.tensor_tensor(out=modrow.t[:, gi * 256:(gi + 1) * 256], in0=pt.t[0:2, 0:256],
                                                  in1=adab.t[:, gi * 256:(gi + 1) * 256], op=ALU.add),
                   R=[pt, adab], W=[modrow])
            store(modrow, MODD[:, :], modrow.t[:])
            for half in range(2):
                pt = nextA()
                for j in range(48):
                    jj = half * 48 + j
                    op(pe, lambda e, j=j, jj=jj: e.transpose(pt.t[:, 2 * j:2 * j + 2], modrow.t[0:2, jj * 128:(jj + 1) * 128],
                                                             ident.t[0:2, 0:2]),
                       R=[modrow, ident], W=[pt], sig=(j == 47))
                op(dve, lambda e: e.tensor_copy(out=modc.t[:, half * 48:(half + 1) * 48, :].rearrange("p j r -> p (j r)"),
                                                in_=pt.t[:, 0:96]), R=[pt], W=[modc])
            kb.barrier()
            n1w = kb.sb([128, 16], F32, e0)
            n2w = kb.sb([128, 16], F32, e0)
            load(n1w, n1w.t[:], n1w_in[:, :])
            load(n2w, n2w.t[:], n2w_in[:, :])
            load(g1B, g1B.t[:], MODD[0:1, 2 * D:3 * D].broadcast_to([128, D]))
            load(g2B, g2B.t[:], MODD[0:1, 5 * D:6 * D].broadcast_to([128, D]))
            for (dst, nw, sec, r) in ((A1, n1w, 1, 0), (Ac1, n1w, 1, 1), (A2, n2w, 4, 0)):
                op(dve, lambda e, dst=dst, nw=nw, sec=sec, r=r: e.scalar_tensor_tensor(
                    out=dst.t[:], in0=modc.t[:, sec * 16:(sec + 1) * 16, r], scalar=1.0, in1=nw.t[:],
                    op0=ALU.add, op1=ALU.mult), R=[modc, nw], W=[dst])
            kb.barrier()

        def sh(sec, r):
            return modc.t[:, sec * 16:(sec + 1) * 16, r]

        with ExitStack() as ew:
            wf = [kb.sb([128, 16, 512], F32, ew) for _ in range(2)]
            wb = [kb.sb([128, 16, 512], BF16, ew) for _ in range(2)]
            snw = kb.sb([128, 32], F32, ew)
            load(snw, snw.t[:], snw_in[:, :])
            cnt = [0]

            def prep(src_ap, dst_ap, nk, ncol, scale_col=None):
                i = cnt[0] % 2
                cnt[0] += 1
                f, b = wf[i], wb[i]
                load(f, f.t[:, 0:nk, 0:ncol], src_ap)
                if scale_col is None:
                    op(pool, lambda e: e.tensor_copy(out=b.t[:, 0:nk, 0:ncol], in_=f.t[:, 0:nk, 0:ncol]), R=[f], W=[b])
                else:
                    for k in range(nk):
                        op(dve, lambda e, k=k: e.tensor_scalar(out=b.t[:, k, 0:ncol], in0=f.t[:, k, 0:ncol],
                                                               scalar1=scale_col(k), scalar2=None, op0=ALU.mult),
                           R=[f, snw], W=[b])
                store(b, dst_ap, b.t[:, 0:nk, 0:ncol])

            for ft in range(16):
                for kh in range(2):
                    prep(wout_in[kh * 2048:(kh + 1) * 2048, ft * 128:(ft + 1) * 128].rearrange("(k p) c -> p k c", p=128),
                         WOUTB[ft, :, kh * 16:(kh + 1) * 16, :], 16, 128,
                         scale_col=lambda k, kh=kh: snw.t[:, kh * 16 + k:kh * 16 + k + 1])
                prep(wcf_in[:, ft * 128:(ft + 1) * 128].rearrange("(k p) c -> p k c", p=128), WCFB[ft, :, :, :], 16, 128)
            for fc in range(4):
                prep(wo_in[:, fc * 512:(fc + 1) * 512].rearrange("(k p) c -> p k c", p=128), WOB[fc, :, :, :], 16, 512)
            for ei in range(EL):
                for kg in range(4):
                    prep(eg_in[ei, :, kg * 256:(kg + 1) * 256].rearrange("(k p) c -> p k c", p=128), EXG[ei, kg, :, :, :], 16, 256)
                    prep(eu_in[ei, :, kg * 256:(kg + 1) * 256].rearrange("(k p) c -> p k c", p=128), EXU[ei, kg, :, :, :], 16, 256)
                for fc in range(4):
                    prep(ed_in[ei, :, fc * 512:(fc + 1) * 512].rearrange("(k p) c -> p k c", p=128), EXD[ei, fc, :, :, :], 8, 512)
            kb.barrier()

        superblocks = [(s0, SBW, False) for s0 in range(0, SEQ, SBW)] + [(0, CTX, True)]
        with ExitStack() as ea:
            hT = kb.sb([128, 16, SBW + 6], BF16, ea)
            xts = [kb.sb([128, D], F32, ea) for _ in range(2)]
            junk = kb.sb([128, D], BF16, ea)
            xh = kb.sb([8, D], F32, ea)
            wfa = [kb.sb([128, 16, 256], F32, ea) for _ in range(2)]
            wba = [kb.sb([128, 16, 256], BF16, ea) for _ in range(2)]
            stage = [kb.sb([128, SBW + 6], F32, ea) for _ in range(2)]
            cacc = kb.sb([128, SBW], F32, ea)
            cob = [kb.sb([128, SBW], BF16, ea) for _ in range(2)]
            sg = [kb.sb([128, 512], F32, ea) for _ in range(2)]
            uo = [kb.sb([128, 512], F32, ea) for _ in range(2)]
            zo = [kb.sb([128, 256], BF16, ea) for _ in range(2)]
            dto = [kb.sb([128, 64], F32, ea) for _ in range(2)]
            cw = kb.sb([128, 48, 7], F32, ea)
            cb = kb.sb([128, 48], F32, ea)
            load(cw, cw.t[:], cw_in[:, :, :])
            load(cb, cb.t[:], cb_in[:, :])
            wc = [0]

            def loadw(c0, ncol):
                i = wc[0] % 2
                wc[0] += 1
                f, b = wfa[i], wba[i]
                load(f, f.t[:, :, 0:ncol], w_in[:, c0:c0 + ncol].rearrange("(k p) c -> p k c", p=128))
                op(pool, lambda e: e.tensor_copy(out=b.t[:, :, 0:ncol], in_=f.t[:, :, 0:ncol]), R=[f], W=[b])
                return b

            for (s0, sw, is_ctx) in superblocks:
                src = ctx_in if is_ctx else x_in
                tok0 = SEQ if is_ctx else s0
                slen = CTX if is_ctx else SEQ
                Acol = Ac1 if is_ctx else A1
                r = 1 if is_ctx else 0
                for ti in range(sw // 128):
                    xt = xts[ti % 2]
                    load(xt, xt.t[:], src[s0 + ti * 128:s0 + (ti + 1) * 128, :])
                    with ExitStack() as el:
                        rs = rms_rstd(el, xt, 128, D, junk)
                        op(dve, lambda e: e.tensor_scalar(out=xt.t[:], in0=xt.t[:], scalar1=rs.t[:, 0:1], scalar2=None,
                                                          op0=ALU.mult), R=[xt, rs], W=[xt])
                    for k4 in range(4):
                        pt = nextA()
                        for kk in range(4):
                            k = k4 * 4 + kk
                            op(pe, lambda e, k=k, kk=kk: e.transpose(pt.t[:, kk * 128:(kk + 1) * 128],
                                                                     xt.t[:, k * 128:(k + 1) * 128], ident.t[:]),
                               R=[xt, ident], W=[pt], sig=(kk == 3))
                        for kk in range(4):
                            k = k4 * 4 + kk
                            op(act, lambda e, k=k, kk=kk: e.activation(
                                out=hT.t[:, k, 3 + ti * 128:3 + (ti + 1) * 128], in_=pt.t[:, kk * 128:(kk + 1) * 128],
                                func=AF.Identity, scale=Acol.t[:, k:k + 1], bias=sh(0, r)[:, k:k + 1]),
                               R=[pt, Acol, modc], W=[hT])
                for side in range(2):
                    lo = s0 - 3 if side == 0 else s0 + sw
                    col0 = 0 if side == 0 else sw + 3
                    if lo < 0 or lo + 3 > slen:
                        op(dve, lambda e, col0=col0: e.memset(hT.t[:, :, col0:col0 + 3], 0.0), W=[hT])
                        continue
                    load(xh, xh.t[0:3, :], src[lo:lo + 3, :])
                    with ExitStack() as el:
                        rs = rms_rstd(el, xh, 3, D, junk)
                        op(dve, lambda e: e.tensor_scalar(out=xh.t[0:3, :], in0=xh.t[0:3, :], scalar1=rs.t[0:3, 0:1],
                                                          scalar2=None, op0=ALU.mult), R=[xh, rs], W=[xh])
                    pt = nextA()
                    for k in range(16):
                        op(pe, lambda e, k=k: e.transpose(pt.t[:, k * 4:k * 4 + 3], xh.t[0:3, k * 128:(k + 1) * 128],
                                                          ident.t[0:3, 0:3]), R=[xh, ident], W=[pt], sig=(k == 15))
                    for k in range(16):
                        op(act, lambda e, k=k, col0=col0: e.activation(
                            out=hT.t[:, k, col0:col0 + 3], in_=pt.t[:, k * 4:k * 4 + 3], func=AF.Identity,
                            scale=Acol.t[:, k:k + 1], bias=sh(0, r)[:, k:k + 1]), R=[pt, Acol, modc], W=[hT])

                chunks = [(c, min(512, sw + 6 - c)) for c in range(0, sw + 6, 512)]
                core_chunks = [(c, min(512, sw - c)) for c in range(0, sw, 512)]

                def mm_fm(wb_, wcol, c, n, off):
                    pt = nextA()
                    for k in range(16):
                        op(pe, lambda e, k=k: e.matmul(pt.t[:, 0:n], lhsT=wb_.t[:, k, wcol:wcol + 128],
                                                       rhs=hT.t[:, k, off + c:off + c + n], start=(k == 0), stop=(k == 15)),
                           R=[wb_, hT], W=[pt], sig=(k == 15))
                    return pt

                for g in range(XBC // 256):
                    wb_ = loadw(g * 256, 256)
                    for tt in range(2):
                        j = g * 2 + tt
                        sg_ = stage[j % 2]
                        for (c, n) in chunks:
                            pt = mm_fm(wb_, tt * 128, c, n, 0)
                            op(act, lambda e, c=c, n=n, pt=pt: e.activation(out=sg_.t[:, c:c + n], in_=pt.t[:, 0:n], func=AF.Copy),
                               R=[pt], W=[sg_])
                        op(dve, lambda e: e.tensor_scalar(out=cacc.t[:, 0:sw], in0=sg_.t[:, 0:sw], scalar1=cw.t[:, j, 0:1],
                                                          scalar2=None, op0=ALU.mult), R=[sg_, cw], W=[cacc])
                        for kk in range(1, 7):
                            op(dve, lambda e, kk=kk: e.scalar_tensor_tensor(
                                out=cacc.t[:, 0:sw], in0=sg_.t[:, kk:kk + sw], scalar=cw.t[:, j, kk:kk + 1], in1=cacc.t[:, 0:sw],
                                op0=ALU.mult, op1=ALU.add), R=[sg_, cw, cacc], W=[cacc])
                        co = cob[j % 2]
                        op(act, lambda e: e.activation(out=co.t[:, 0:sw], in_=cacc.t[:, 0:sw], func=AF.Silu, bias=cb.t[:, j:j + 1]),
                           R=[cacc, cb], W=[co])
                        store(co, XBCs[j * 128:(j + 1) * 128, tok0:tok0 + sw], co.t[:, 0:sw])
                wb_ = loadw(OFF_DT, 64)
                for ti in range(sw // 128):
                    pt = nextA()
                    for k in range(16):
                        op(pe, lambda e, k=k: e.matmul(pt.t[:, 0:64], lhsT=hT.t[:, k, 3 + ti * 128:3 + (ti + 1) * 128],
                                                       rhs=wb_.t[:, k, 0:64], start=(k == 0), stop=(k == 15)),
                           R=[wb_, hT], W=[pt], sig=(k == 15))
                    do = dto[ti % 2]
                    op(act, lambda e: e.activation(out=do.t[:], in_=pt.t[:, 0:64], func=AF.Copy), R=[pt], W=[do])
                    store(do, DTs[tok0 + ti * 128:tok0 + (ti + 1) * 128, :], do.t[:])
                if is_ctx:
                    continue
                for g in range(16):
                    wb_ = loadw(OFF_Z + g * 256, 256)
                    for ti in range(sw // 128):
                        pt = nextA()
                        for k in range(16):
                            op(pe, lambda e, k=k: e.matmul(pt.t[:, 0:256], lhsT=hT.t[:, k, 3 + ti * 128:3 + (ti + 1) * 128],
                                                           rhs=wb_.t[:, k, 0:256], start=(k == 0), stop=(k == 15)),
                               R=[wb_, hT], W=[pt], sig=(k == 15))
                        z_ = zo[ti % 2]
                        op(act, lambda e: e.activation(out=z_.t[:], in_=pt.t[:, 0:256], func=AF.Silu), R=[pt], W=[z_])
                        store(z_, ZS[s0 + ti * 128:s0 + (ti + 1) * 128, g * 256:(g + 1) * 256], z_.t[:])
                for g in range(8):
                    wa = loadw(OFF_GLU + g * 256, 256)
                    wbb = loadw(OFF_GLU + D + g * 256, 256)
                    for tt in range(2):
                        j = g * 2 + tt
                        for (c, n) in core_chunks:
                            pa = mm_fm(wa, tt * 128, c, n, 3)
                            pb = mm_fm(wbb, tt * 128, c, n, 3)
                            s_ = sg[(c // 512) % 2]
                            u_ = uo[(c // 512) % 2]
                            op(act, lambda e, n=n, pb=pb: e.activation(out=s_.t[:, 0:n], in_=pb.t[:, 0:n], func=AF.Sigmoid),
                               R=[pb], W=[s_])
                            op(dve, lambda e, n=n, pa=pa: e.tensor_tensor(out=u_.t[:, 0:n], in0=pa.t[:, 0:n], in1=s_.t[:, 0:n],
                                                                          op=ALU.mult), R=[pa, s_], W=[u_])
                            store(u_, U[j * 128:(j + 1) * 128, s0 + c:s0 + c + n], u_.t[:, 0:n])
                for g in range(16):
                    wb_ = loadw(OFF_GATE + g * 256, 256)
                    for tt in range(2):
                        j = g * 2 + tt
                        co = cob[j % 2]
                        for (c, n) in core_chunks:
                            pt = mm_fm(wb_, tt * 128, c, n, 3)
                            op(act, lambda e, c=c, n=n, pt=pt: e.activation(out=co.t[:, c:c + n], in_=pt.t[:, 0:n], func=AF.Sigmoid),
                               R=[pt], W=[co])
                        store(co, GT[j * 128:(j + 1) * 128, s0:s0 + sw], co.t[:, 0:sw])
            kb.barrier()

        with ExitStack() as eb:
            H = kb.sb([128, DI], F32, eb)
            Hb = kb.sb([128, DI], BF16, eb)
            xsF = [kb.sb([128, 32, 128], BF16, eb) for _ in range(2)]
            BF_ = [kb.sb([128, 8, 128], BF16, eb) for _ in range(2)]
            CF_ = [kb.sb([128, 8, 128], BF16, eb) for _ in range(2)]
            dtr = [kb.sb([128, 64], F32, eb) for _ in range(2)]
            xs_tok = kb.sb([128, DI], BF16, eb)
            B_tok = kb.sb([128, 1024], BF16, eb)
            xdt = kb.sb([128, DI], BF16, eb)
            xdtw = kb.sb([128, DI], BF16, eb)
            ybuf = [kb.sb([128, DI], F32, eb) for _ in range(2)]
            yfl = kb.sb([128, DI], F32, eb)
            dt_ = kb.sb([128, 64], F32, eb)
            dA = kb.sb([128, 64], F32, eb)
            ncum = kb.sb([128, 64], F32, eb)
            ecum = kb.sb([128, 64], F32, eb)
            toend = kb.sb([128, 64], F32, eb)
            cd = kb.sb([128, 64], F32, eb)
            dtw = kb.sb([128, 64], F32, eb)
            Em = [kb.sb([128, 128], F32, eb) for _ in range(2)]
            MT = [kb.sb([128, 128], BF16, eb) for _ in range(3)]
            t1 = kb.sb([128, 512], F32, eb)
            t3 = kb.sb([128, 512], F32, eb)
            hts = kb.sb([128, 512], F32, eb)
            dtb = kb.sb([128, 2, 64], F32, eb)
            aneg = kb.sb([128, 2, 64], F32, eb)
            dsk = kb.sb([128, 2, 64], F32, eb)
            load(dtb, dtb.t[:], dtb_in[:, :, :])
            load(aneg, aneg.t[:], alog_in[:, :, :])
            load(dsk, dsk.t[:], dsk_in[:, :, :])
            op(act, lambda e: e.activation(out=aneg.t[:], in_=aneg.t[:], func=AF.Exp), R=[aneg], W=[aneg])
            op(dve, lambda e: e.tensor_scalar(out=aneg.t[:], in0=aneg.t[:], scalar1=-1.0, scalar2=None, op0=ALU.mult),
               R=[aneg], W=[aneg])

            def bc8(ap8):
                return ap8.rearrange("p (h o) -> p h o", o=1).broadcast_to([128, 8, 64])

            ci = [0]
            for dr in range(2):
                op(dve, lambda e: e.memset(H.t[:], 0.0), W=[H])
                op(dve, lambda e: e.memset(Hb.t[:], 0.0), W=[Hb])
                seq_chunks = [(SEQ + c * 128, False) for c in range(CTX // 128)] + [(c * 128, True) for c in range(NT)]
                if dr == 1:
                    seq_chunks = [(SEQ + c * 128, False) for c in reversed(range(CTX // 128))] + \
                                 [(c * 128, True) for c in reversed(range(NT))]
                for (t0, with_y) in seq_chunks:
                    i = ci[0] % 2
                    ci[0] += 1
                    xf, bf, cf, dtr_ = xsF[i], BF_[i], CF_[i], dtr[i]
                    load(xf, xf.t[:], XBCs[0:DI, t0:t0 + 128].rearrange("(j p) t -> p j t", p=128))
                    load(bf, bf.t[:], XBCs[DI:DI + 1024, t0:t0 + 128].rearrange("(j p) t -> p j t", p=128))
                    if with_y:
                        load(cf, cf.t[:], XBCs[DI + 1024:XBC, t0:t0 + 128].rearrange("(j p) t -> p j t", p=128))
                    load(dtr_, dtr_.t[:], DTs[t0:t0 + 128, :])
                    op(dve, lambda e: e.tensor_tensor(out=dt_.t[:], in0=dtr_.t[:], in1=dtb.t[:, dr, :], op=ALU.add),
                       R=[dtr_, dtb], W=[dt_])
                    op(act, lambda e: e.activation(out=dt_.t[:], in_=dt_.t[:], func=AF.Exp), R=[dt_], W=[dt_])
                    op(act, lambda e: e.activation(out=dt_.t[:], in_=dt_.t[:], func=AF.Ln, bias=onec.t[:, 0:1]), R=[dt_, onec], W=[dt_])
                    op(dve, lambda e: e.tensor_tensor(out=dA.t[:], in0=dt_.t[:], in1=aneg.t[:, dr, :], op=ALU.mult),
                       R=[dt_, aneg], W=[dA])
                    op(pe, lambda e: e.matmul(psS.t[:, 0:64], lhsT=tri.t[:, dr, :], rhs=dA.t[:], start=True, stop=True),
                       R=[tri, dA], W=[psS], sig=False)
                    op(pe, lambda e: e.matmul(psS.t[:, 64:128], lhsT=ones.t[:], rhs=dA.t[:], start=True, stop=True),
                       R=[ones, dA], W=[psS])
                    op(act, lambda e: e.activation(out=ncum.t[:], in_=psS.t[:, 0:64], func=AF.Identity, scale=-1.0), R=[psS], W=[ncum])
                    op(act, lambda e: e.activation(out=ecum.t[:], in_=psS.t[:, 0:64], func=AF.Exp), R=[psS], W=[ecum])
                    op(act, lambda e: e.activation(out=cd.t[:], in_=psS.t[:, 64:128], func=AF.Exp), R=[psS], W=[cd])
                    op(dve, lambda e: e.tensor_tensor(out=toend.t[:], in0=psS.t[:, 64:128], in1=ncum.t[:], op=ALU.add),
                       R=[psS, ncum], W=[toend])
                    op(act, lambda e: e.activation(out=toend.t[:], in_=toend.t[:], func=AF.Exp), R=[toend], W=[toend])
                    op(dve, lambda e: e.tensor_tensor(out=dtw.t[:], in0=dt_.t[:], in1=toend.t[:], op=ALU.mult),
                       R=[dt_, toend], W=[dtw])
                    for q8 in range(4):
                        pt = nextT()
                        for jj in range(8):
                            j = q8 * 8 + jj
                            op(pe, lambda e, j=j, jj=jj: e.transpose(pt.t[:, jj * 128:(jj + 1) * 128], xf.t[:, j, :], identb.t[:]),
                               R=[xf, identb], W=[pt], sig=(jj == 7))
                        op(act, lambda e, q8=q8: e.activation(out=xs_tok.t[:, q8 * 1024:(q8 + 1) * 1024], in_=pt.t[:], func=AF.Copy),
                           R=[pt], W=[xs_tok])
                    pt = nextT()
                    for jj in range(8):
                        op(pe, lambda e, jj=jj: e.transpose(pt.t[:, jj * 128:(jj + 1) * 128], bf.t[:, jj, :], identb.t[:]),
                           R=[bf, identb], W=[pt], sig=(jj == 7))
                    op(act, lambda e: e.activation(out=B_tok.t[:], in_=pt.t[:], func=AF.Copy), R=[pt], W=[B_tok])
                    v3 = lambda tt: tt.t[:].rearrange("p (h q) -> p h q", q=64)
                    if with_y:
                        op(dve, lambda e: e.tensor_tensor(out=v3(xdt), in0=v3(xs_tok),
                                                          in1=dt_.t[:].rearrange("p (h o) -> p h o", o=1).broadcast_to([128, 64, 64]),
                                                          op=ALU.mult), R=[xs_tok, dt_], W=[xdt])
                    op(dve, lambda e: e.tensor_tensor(out=v3(xdtw), in0=v3(xs_tok),
                                                      in1=dtw.t[:].rearrange("p (h o) -> p h o", o=1).broadcast_to([128, 64, 64]),
                                                      op=ALU.mult), R=[xs_tok, dtw], W=[xdtw])
                    yb = ybuf[i]
                    if with_y and dr == 1:
                        load(yfl, yfl.t[:], Y[t0:t0 + 128, :])
                    for g in range(NG):
                        gs = slice(g * 512, (g + 1) * 512)
                        if with_y:
                            psc = psX
                            op(pe, lambda e: e.matmul(psc.t[:, 0:128], lhsT=bf.t[:, g, :], rhs=cf.t[:, g, :], start=True, stop=True),
                               R=[bf, cf], W=[psc])
                            pyd = psY
                            for hh in range(8):
                                h = g * 8 + hh
                                p2 = nextA()
                                op(pe, lambda e, h=h: e.matmul(p2.t[:, 0:128], lhsT=dA.t[:, h:h + 1].broadcast_to([128, 128]),
                                                               rhs=tri.t[:, dr, :], start=True, stop=False),
                                   R=[dA, tri], W=[p2], sig=False)
                                op(pe, lambda e: e.matmul(p2.t[:, 0:128], lhsT=ident.t[:], rhs=nmask.t[:, dr, :], start=False, stop=True),
                                   R=[ident, nmask], W=[p2])
                                em = Em[h % 2]
                                op(act, lambda e, h=h: e.activation(out=em.t[:], in_=p2.t[:, 0:128], func=AF.Exp, bias=ncum.t[:, h:h + 1]),
                                   R=[p2, ncum], W=[em])
                                mt = MT[h % 3]
                                op(dve, lambda e: e.tensor_tensor(out=mt.t[:], in0=psc.t[:, 0:128], in1=em.t[:], op=ALU.mult),
                                   R=[psc, em], W=[mt])
                                op(pe, lambda e, h=h, hh=hh: e.matmul(pyd.t[:, hh * 64:(hh + 1) * 64], lhsT=mt.t[:],
                                                                      rhs=xdt.t[:, h * 64:(h + 1) * 64], start=True, stop=True),
                                   R=[mt, xdt], W=[pyd], sig=(hh == 7))
                            pyo = nextA()
                            op(pe, lambda e: e.matmul(pyo.t[:], lhsT=cf.t[:, g, :], rhs=Hb.t[:, gs], start=True, stop=True),
                               R=[cf, Hb], W=[pyo])
                            r3 = lambda ap: ap.rearrange("p (h q) -> p h q", q=64)
                            op(dve, lambda e: e.tensor_tensor(out=r3(t1.t[:]), in0=r3(pyo.t[:]), in1=bc8(ecum.t[:, g * 8:(g + 1) * 8]),
                                                              op=ALU.mult), R=[pyo, ecum], W=[t1])
                            op(dve, lambda e: e.tensor_tensor(out=t1.t[:], in0=t1.t[:], in1=pyd.t[:], op=ALU.add), R=[t1, pyd], W=[t1])
                            op(dve, lambda e: e.tensor_tensor(out=r3(t3.t[:]), in0=r3(xs_tok.t[:, gs]),
                                                              in1=bc8(dsk.t[:, dr, g * 8:(g + 1) * 8]), op=ALU.mult),
                               R=[xs_tok, dsk], W=[t3])
                            if dr == 0:
                                op(dve, lambda e: e.tensor_tensor(out=yb.t[:, gs], in0=t1.t[:], in1=t3.t[:], op=ALU.add),
                                   R=[t1, t3], W=[yb])
                            else:
                                op(dve, lambda e: e.tensor_tensor(out=t1.t[:], in0=t1.t[:], in1=t3.t[:], op=ALU.add), R=[t1, t3], W=[t1])
                                op(dve, lambda e: e.tensor_tensor(out=yb.t[:, gs], in0=t1.t[:], in1=yfl.t[:, gs], op=ALU.add),
                                   R=[t1, yfl], W=[yb])
                        pst = nextA()
                        op(pe, lambda e: e.matmul(pst.t[:], lhsT=B_tok.t[:, g * 128:(g + 1) * 128], rhs=xdtw.t[:, gs], start=True, stop=True),
                           R=[B_tok, xdtw], W=[pst])
                        r3 = lambda ap: ap.rearrange("p (h q) -> p h q", q=64)
                        op(dve, lambda e: e.tensor_tensor(out=r3(hts.t[:]), in0=r3(H.t[:, gs]), in1=bc8(cd.t[:, g * 8:(g + 1) * 8]),
                                                          op=ALU.mult), R=[H, cd], W=[hts])
                        op(dve, lambda e: e.tensor_tensor(out=H.t[:, gs], in0=hts.t[:], in1=pst.t[:], op=ALU.add), R=[hts, pst], W=[H])
                        op(act, lambda e: e.activation(out=Hb.t[:, gs], in_=H.t[:, gs], func=AF.Copy), R=[H], W=[Hb])
                    if with_y:
                        store(yb, Y[t0:t0 + 128, :], yb.t[:])
                kb.barrier()

        with ExitStack() as ec:
            upad = kb.sb([128, SEQ + 2 * PAD], F32, ec)
            vacc = kb.sb([128, SEQ], F32, ec)
            cfw = kb.sb([128, 16, 31], F32, ec)
            cfb = kb.sb([128, 16], F32, ec)
            load(cfw, cfw.t[:], cfw_in[:, :, :])
            load(cfb, cfb.t[:], cfb_in[:, :])
            op(dve, lambda e: e.memset(upad.t[:, 0:PAD], 0.0), W=[upad])
            op(dve, lambda e: e.memset(upad.t[:, PAD + SEQ:], 0.0), W=[upad])
            for j in range(16):
                load(upad, upad.t[:, PAD:PAD + SEQ], U[j * 128:(j + 1) * 128, :])
                op(dve, lambda e, j=j: e.tensor_scalar(out=vacc.t[:], in0=upad.t[:, 0:SEQ], scalar1=cfw.t[:, j, 0:1],
                                                       scalar2=cfb.t[:, j:j + 1], op0=ALU.mult, op1=ALU.add),
                   R=[upad, cfw, cfb], W=[vacc])
                for kk in range(1, 31):
                    op(dve, lambda e, j=j, kk=kk: e.scalar_tensor_tensor(
                        out=vacc.t[:], in0=upad.t[:, kk * GRID_W:kk * GRID_W + SEQ], scalar=cfw.t[:, j, kk:kk + 1], in1=vacc.t[:],
                        op0=ALU.mult, op1=ALU.add), R=[upad, cfw, vacc], W=[vacc])
                store(vacc, V[j * 128:(j + 1) * 128, :], vacc.t[:])
            kb.barrier()

        BLK = 256
        with ExitStack() as ec:
            ygT = kb.sb([128, 32, BLK], BF16, ec)
            uT = kb.sb([128, 16, BLK], BF16, ec)
            mT = kb.sb([128, 16, BLK], BF16, ec)
            vk = [kb.sb([128, BLK], F32, ec) for _ in range(2)]
            gA = [kb.sb([128, BLK], BF16, ec) for _ in range(2)]
            gB = [kb.sb([128, BLK], BF16, ec) for _ in range(2)]
            yt = kb.sb([128, DI], F32, ec)
            zt = kb.sb([128, DI], BF16, ec)
            yzn = kb.sb([128, DI], BF16, ec)
            junk = kb.sb([128, DI], BF16, ec)
            wo_t = [kb.sb([128, 32, 128], BF16, ec) for _ in range(2)]
            wc_t = [kb.sb([128, 16, 128], BF16, ec) for _ in range(2)]
            wob = [kb.sb([128, 16, 512], BF16, ec) for _ in range(2)]
            mean = kb.sb([128, BLK], F32, ec)
            rstd = kb.sb([128, BLK], F32, ec)
            sq = kb.sb([128, BLK], F32, ec)
            tmp = kb.sb([128, 512], F32, ec)
            tm1 = kb.sb([128, BLK], F32, ec)
            tm2 = kb.sb([128, BLK], F32, ec)
            xt = kb.sb([128, D], F32, ec)
            x2 = kb.sb([128, D], F32, ec)
            h32 = kb.sb([128, 16, 128], F32, ec)
            h2b = [kb.sb([128, 16, 128], BF16, ec) for _ in range(2)]
            lnw = kb.sb([128, 16], F32, ec)
            lnb = kb.sb([128, 16], F32, ec)
            cfob = kb.sb([128, 16], F32, ec)
            rw = kb.sb([128, 16, 72], F32, ec)
            rb = kb.sb([128, 72], F32, ec)
            gsel = kb.sb([128, 2, 8], F32, ec)
            L = kb.sb([128, 72], F32, ec)
            sm = kb.sb([128, 64], F32, ec)
            ohg = kb.sb([128, 8], F32, ec)
            eg3 = kb.sb([128, 64], F32, ec)
            ein = kb.sb([128, 8], F32, ec)
            oh1 = kb.sb([128, 8], F32, ec)
            oh2 = kb.sb([128, 8], F32, ec)
            ce = kb.sb([128, 8], F32, ec)
            ohl = kb.sb([128, 2], F32, ec)
            for (tt_, src_) in ((lnw, lnw_in), (lnb, lnb_in), (cfob, cfob_in), (rb, rb_in)):
                load(tt_, tt_.t[:], src_[:, :])
            load(rw, rw.t[:], rw_in[:, :, :])
            load(gsel, gsel.t[:], gsel_in[:, :, :])
            wi = [0]
            vi = [0]

            def loadv(k, b0):
                v_ = vk[vi[0] % 2]
                vi[0] += 1
                load(v_, v_.t[:], V[k * 128:(k + 1) * 128, b0:b0 + BLK])
                return v_

            for blk in range(SEQ // BLK):
                b0 = blk * BLK
                NTB = BLK // 128
                for ti in range(NTB):
                    r0 = b0 + ti * 128
                    load(yt, yt.t[:], Y[r0:r0 + 128, :])
                    load(zt, zt.t[:], ZS[r0:r0 + 128, :])
                    op(dve, lambda e: e.tensor_tensor(out=yt.t[:], in0=yt.t[:], in1=zt.t[:], op=ALU.mult), R=[yt, zt], W=[yt])
                    with ExitStack() as el:
                        rs = rms_rstd(el, yt, 128, DI, junk)
                        op(act, lambda e: e.activation(out=yzn.t[:], in_=yt.t[:], func=AF.Identity, scale=rs.t[:, 0:1]), R=[yt, rs], W=[yzn])
                    for q8 in range(4):
                        pt = nextT()
                        for jj in range(8):
                            j = q8 * 8 + jj
                            op(pe, lambda e: e.transpose(pt.t[:, jj * 128:(jj + 1) * 128], yzn.t[:, j * 128:(j + 1) * 128],
                                                         identb.t[:]), R=[yzn, identb], W=[pt], sig=(jj == 7))
                        op(act, lambda e: e.activation(
                            out=ygT.t[:, q8 * 8:(q8 + 1) * 8, ti * 128:(ti + 1) * 128],
                            in_=pt.t[:].rearrange("p (j t) -> p j t", t=128), func=AF.Copy), R=[pt], W=[ygT])
                ps1 = nextA()
                for k in range(16):
                    v_ = loadv(k, b0)
                    op(pe, lambda e: e.matmul(ps1.t[:, 0:BLK], lhsT=ones.t[:], rhs=v_.t[:], start=(k == 0), stop=(k == 15)),
                       R=[ones, v_], W=[ps1], sig=True)
                op(act, lambda e: e.activation(out=mean.t[:], in_=ps1.t[:, 0:BLK], func=AF.Identity, scale=1.0 / D), R=[ps1], W=[mean])
                ps2 = nextA()
                for k in range(16):
                    v_ = loadv(k, b0)
                    op(dve, lambda e: e.tensor_tensor(out=sq.t[:], in0=v_.t[:], in1=mean.t[:], op=ALU.subtract),
                       R=[v_, mean], W=[sq])
                    op(act, lambda e: e.activation(out=sq.t[:], in_=sq.t[:], func=AF.Square), R=[sq], W=[sq])
                    op(pe, lambda e: e.matmul(ps2.t[:, 0:BLK], lhsT=ones.t[:], rhs=sq.t[:], start=(k == 0), stop=(k == 15)),
                       R=[ones, sq], W=[ps2])
                op(act, lambda e: e.activation(out=rstd.t[:], in_=ps2.t[:, 0:BLK], func=AF.Sqrt, scale=1.0 / D, bias=epsc.t[:, 0:1]),
                   R=[ps2, epsc], W=[rstd])
                op(dve, lambda e: e.reciprocal(out=rstd.t[:], in_=rstd.t[:]), R=[rstd], W=[rstd])
                for k in range(16):
                    v_ = loadv(k, b0)
                    op(dve, lambda e: e.tensor_tensor(out=tm1.t[:], in0=v_.t[:], in1=mean.t[:], op=ALU.subtract),
                       R=[v_, mean], W=[tm1])
                    op(dve, lambda e: e.tensor_tensor(out=tm1.t[:], in0=tm1.t[:], in1=rstd.t[:], op=ALU.mult), R=[tm1, rstd], W=[tm1])
                    op(act, lambda e: e.activation(out=uT.t[:, k, :], in_=tm1.t[:], func=AF.Silu, scale=lnw.t[:, k:k + 1],
                                                   bias=lnb.t[:, k:k + 1]), R=[tm1, lnw, lnb], W=[uT])
                for ft in range(16):
                    i = wi[0] % 2
                    wi[0] += 1
                    w1, w2, ga, gb = wo_t[i], wc_t[i], gA[i], gB[i]
                    load(w1, w1.t[:], WOUTB[ft, :, :, :])
                    load(w2, w2.t[:], WCFB[ft, :, :, :])
                    load(ga, ga.t[:], GT[ft * 128:(ft + 1) * 128, b0:b0 + BLK])
                    load(gb, gb.t[:], GT[D + ft * 128:D + (ft + 1) * 128, b0:b0 + BLK])
                    pss = nextA()
                    for k in range(32):
                        op(pe, lambda e: e.matmul(pss.t[:, 0:BLK], lhsT=w1.t[:, k, :], rhs=ygT.t[:, k, :], start=(k == 0), stop=(k == 31)),
                           R=[w1, ygT], W=[pss], sig=(k == 31))
                    psc = nextA()
                    for k in range(16):
                        op(pe, lambda e: e.matmul(psc.t[:, 0:BLK], lhsT=w2.t[:, k, :], rhs=uT.t[:, k, :], start=(k == 0), stop=(k == 15)),
                           R=[w2, uT], W=[psc], sig=(k == 15))
                    op(dve, lambda e: e.tensor_tensor(out=tm1.t[:], in0=pss.t[:, 0:BLK], in1=ga.t[:], op=ALU.mult),
                       R=[pss, ga], W=[tm1])
                    op(dve, lambda e: e.scalar_tensor_tensor(out=tm2.t[:], in0=psc.t[:, 0:BLK], scalar=cfob.t[:, ft:ft + 1],
                                                             in1=gb.t[:], op0=ALU.add, op1=ALU.mult),
                       R=[psc, cfob, gb], W=[tm2])
                    op(dve, lambda e: e.tensor_tensor(out=mT.t[:, ft, :], in0=tm1.t[:], in1=tm2.t[:], op=ALU.add),
                       R=[tm1, tm2], W=[mT])
                for ti in range(NTB):
                    r0 = b0 + ti * 128
                    tix = r0 // 128
                    load(xt, xt.t[:], x_in[r0:r0 + 128, :])
                    for fc in range(4):
                        w3 = wob[fc % 2]
                        load(w3, w3.t[:], WOB[fc, :, :, :])
                        po = nextA()
                        for k in range(16):
                            op(pe, lambda e: e.matmul(po.t[:], lhsT=mT.t[:, k, ti * 128:(ti + 1) * 128], rhs=w3.t[:, k, :],
                                                      start=(k == 0), stop=(k == 15)), R=[mT, w3], W=[po], sig=(k == 15))
                        fs = slice(fc * 512, (fc + 1) * 512)
                        op(dve, lambda e: e.tensor_tensor(out=tmp.t[:], in0=po.t[:], in1=g1B.t[:, fs], op=ALU.mult),
                           R=[po, g1B], W=[tmp])
                        op(dve, lambda e: e.tensor_tensor(out=x2.t[:, fs], in0=tmp.t[:], in1=xt.t[:, fs], op=ALU.add),
                           R=[tmp, xt], W=[x2])
                    store(x2, X2[r0:r0 + 128, :], x2.t[:])
                    with ExitStack() as el:
                        rs = rms_rstd(el, x2, 128, D, junk)
                        op(dve, lambda e: e.tensor_scalar(out=xt.t[:], in0=x2.t[:], scalar1=rs.t[:, 0:1], scalar2=None, op0=ALU.mult),
                           R=[x2, rs], W=[xt])
                    for k4 in range(4):
                        pt = nextA()
                        for kk in range(4):
                            k = k4 * 4 + kk
                            op(pe, lambda e: e.transpose(pt.t[:, kk * 128:(kk + 1) * 128], xt.t[:, k * 128:(k + 1) * 128],
                                                         ident.t[:]), R=[xt, ident], W=[pt], sig=(kk == 3))
                        for kk in range(4):
                            k = k4 * 4 + kk
                            op(act, lambda e: e.activation(out=h32.t[:, k, :], in_=pt.t[:, kk * 128:(kk + 1) * 128],
                                                           func=AF.Identity, scale=A2.t[:, k:k + 1], bias=sh(3, 0)[:, k:k + 1]),
                               R=[pt, A2, modc], W=[h32])
                    hb_ = h2b[ti % 2]
                    op(dve, lambda e: e.tensor_copy(out=hb_.t[:], in_=h32.t[:]), R=[h32], W=[hb_])
                    store(hb_, H2T[:, r0:r0 + 128].rearrange("(k p) t -> p k t", p=128), hb_.t[:])
                    pl = nextA()
                    for k in range(16):
                        op(pe, lambda e: e.matmul(pl.t[:, 0:72], lhsT=h32.t[:, k, :], rhs=rw.t[:, k, :], start=(k == 0), stop=(k == 15)),
                           R=[h32, rw], W=[pl], sig=(k == 15))
                    op(dve, lambda e: e.tensor_tensor(out=L.t[:], in0=pl.t[:, 0:72], in1=rb.t[:], op=ALU.add), R=[pl, rb], W=[L])
                    S = lambda a, b=None: sm.t[:, a:(a + 1 if b is None else b)]
                    op(dve, lambda e: e.tensor_reduce(out=S(0), in_=L.t[:, 0:8], axis=AX.X, op=ALU.max), R=[L], W=[sm])
                    op(dve, lambda e: e.tensor_scalar(out=S(8, 16), in0=L.t[:, 0:8], scalar1=S(0), scalar2=None, op0=ALU.subtract),
                       R=[L, sm], W=[sm])
                    op(dve, lambda e: e.tensor_scalar(out=ohg.t[:], in0=S(8, 16), scalar1=0.0, scalar2=None, op0=ALU.is_ge), R=[sm], W=[ohg])
                    op(act, lambda e: e.activation(out=S(8, 16), in_=S(8, 16), func=AF.Exp, accum_out=S(1)), R=[sm], W=[sm])
                    op(dve, lambda e: e.reciprocal(out=S(2), in_=S(1)), R=[sm], W=[sm])
                    op(dve, lambda e: e.tensor_tensor(out=eg3.t[:].rearrange("p (g q) -> p g q", q=8),
                                                      in0=L.t[:, 8:72].rearrange("p (g q) -> p g q", q=8),
                                                      in1=ohg.t[:].rearrange("p (g o) -> p g o", o=1).broadcast_to([128, 8, 8]),
                                                      op=ALU.mult), R=[L, ohg], W=[eg3])
                    op(dve, lambda e: e.tensor_reduce(out=ein.t[:], in_=eg3.t[:].rearrange("p (g q) -> p q g", q=8), axis=AX.X,
                                                      op=ALU.add), R=[eg3], W=[ein])
                    op(dve, lambda e: e.tensor_reduce(out=S(3), in_=ein.t[:], axis=AX.X, op=ALU.max), R=[ein], W=[sm])
                    op(dve, lambda e: e.tensor_scalar(out=oh1.t[:], in0=ein.t[:], scalar1=S(3), scalar2=0.0, op0=ALU.subtract,
                                                      op1=ALU.is_ge), R=[ein, sm], W=[oh1])
                    op(dve, lambda e: e.scalar_tensor_tensor(out=S(16, 24), in0=oh1.t[:], scalar=-1.0e9, in1=ein.t[:],
                                                             op0=ALU.mult, op1=ALU.add), R=[oh1, ein], W=[sm])
                    op(dve, lambda e: e.tensor_reduce(out=S(4), in_=S(16, 24), axis=AX.X, op=ALU.max), R=[sm], W=[sm])
                    op(dve, lambda e: e.tensor_scalar(out=oh2.t[:], in0=S(16, 24), scalar1=S(4), scalar2=0.0, op0=ALU.subtract,
                                                      op1=ALU.is_ge), R=[sm], W=[oh2])
                    op(dve, lambda e: e.tensor_tensor(out=S(5), in0=S(4), in1=S(3), op=ALU.subtract), R=[sm], W=[sm])
                    op(act, lambda e: e.activation(out=S(5), in_=S(5), func=AF.Exp), R=[sm], W=[sm])
                    op(dve, lambda e: e.tensor_scalar(out=S(5), in0=S(5), scalar1=1.0, scalar2=None, op0=ALU.add), R=[sm], W=[sm])
                    op(dve, lambda e: e.reciprocal(out=S(6), in_=S(5)), R=[sm], W=[sm])
                    op(dve, lambda e: e.tensor_scalar(out=S(7), in0=S(6), scalar1=-1.0, scalar2=1.0, op0=ALU.mult, op1=ALU.add),
                       R=[sm], W=[sm])
                    op(dve, lambda e: e.tensor_tensor(out=S(6), in0=S(6), in1=S(2), op=ALU.mult), R=[sm], W=[sm])
                    op(dve, lambda e: e.tensor_tensor(out=S(7), in0=S(7), in1=S(2), op=ALU.mult), R=[sm], W=[sm])
                    op(dve, lambda e: e.tensor_scalar(out=ce.t[:], in0=oh1.t[:], scalar1=S(6), scalar2=None, op0=ALU.mult),
                       R=[oh1, sm], W=[ce])
                    op(dve, lambda e: e.scalar_tensor_tensor(out=ce.t[:], in0=oh2.t[:], scalar=S(7), in1=ce.t[:], op0=ALU.mult,
                                                             op1=ALU.add), R=[oh2, sm, ce], W=[ce])
                    for gl in range(2):
                        op(dve, lambda e: e.tensor_tensor(out=S(24, 32), in0=ohg.t[:], in1=gsel.t[:, gl, :], op=ALU.mult),
                           R=[ohg, gsel], W=[sm])
                        op(dve, lambda e: e.tensor_reduce(out=ohl.t[:, gl:gl + 1], in_=S(24, 32), axis=AX.X, op=ALU.add),
                           R=[sm], W=[ohl])
                        op(dve, lambda e: e.tensor_scalar(out=coef.t[:, tix, gl * 8:(gl + 1) * 8], in0=ce.t[:],
                                                          scalar1=ohl.t[:, gl:gl + 1], scalar2=None, op0=ALU.mult),
                           R=[ce, ohl], W=[coef])
                    op(dve, lambda e: e.tensor_tensor(out=own.t[:, tix:tix + 1], in0=ohl.t[:, 0:1], in1=ohl.t[:, 1:2], op=ALU.add),
                       R=[ohl], W=[own])
            store(own, own_d[:, :], own.t[:])
            kb.barrier()

        with ExitStack() as ed:
            h2 = kb.sb([128, 16, MB], BF16, ed)
            acc = kb.sb([128, MB // 128, D], F32, ed)
            hid = kb.sb([128, 8, MB], BF16, ed)
            wg = [kb.sb([128, 16, 256], BF16, ed) for _ in range(2)]
            wu = [kb.sb([128, 16, 256], BF16, ed) for _ in range(2)]
            wd = [kb.sb([128, 8, 512], BF16, ed) for _ in range(2)]
            sgt = [kb.sb([128, 512], F32, ed) for _ in range(2)]
            x2t = kb.sb([128, D], F32, ed)
            junk = kb.sb([128, D], BF16, ed)
            fnw = kb.sb([128, D], F32, ed)
            load(fnw, fnw.t[:], fnw_in[:, :])
            wi = [0]
            for mb in range(SEQ // MB):
                m0 = mb * MB
                load(h2, h2.t[:], H2T[:, m0:m0 + MB].rearrange("(k p) t -> p k t", p=128))
                op(dve, lambda e: e.memset(acc.t[:], 0.0), W=[acc])
                for ei in range(EL):
                    for kg in range(4):
                        i = wi[0] % 2
                        wi[0] += 1
                        g_, u_ = wg[i], wu[i]
                        load(g_, g_.t[:], EXG[ei, kg, :, :, :])
                        load(u_, u_.t[:], EXU[ei, kg, :, :, :])
                        for kt in range(2):
                            kk = kg * 2 + kt
                            for c in range(0, MB, 512):
                                pg = nextA()
                                for k in range(16):
                                    op(pe, lambda e: e.matmul(pg.t[:], lhsT=g_.t[:, k, kt * 128:(kt + 1) * 128], rhs=h2.t[:, k, c:c + 512],
                                                              start=(k == 0), stop=(k == 15)), R=[g_, h2], W=[pg], sig=(k == 15))
                                pu = nextA()
                                for k in range(16):
                                    op(pe, lambda e: e.matmul(pu.t[:], lhsT=u_.t[:, k, kt * 128:(kt + 1) * 128], rhs=h2.t[:, k, c:c + 512],
                                                              start=(k == 0), stop=(k == 15)), R=[u_, h2], W=[pu], sig=(k == 15))
                                s_ = sgt[(c // 512) % 2]
                                op(act, lambda e: e.activation(out=s_.t[:], in_=pg.t[:], func=AF.Silu), R=[pg], W=[s_])
                                op(dve, lambda e: e.tensor_tensor(out=hid.t[:, kk, c:c + 512], in0=pu.t[:], in1=s_.t[:], op=ALU.mult),
                                   R=[pu, s_], W=[hid])
                    for fc in range(4):
                        d_ = wd[fc % 2]
                        load(d_, d_.t[:], EXD[ei, fc, :, :, :])
                        for ti in range(MB // 128):
                            tix = (m0 + ti * 128) // 128
                            pd = nextA()
                            for k in range(8):
                                op(pe, lambda e: e.matmul(pd.t[:], lhsT=hid.t[:, k, ti * 128:(ti + 1) * 128], rhs=d_.t[:, k, :],
                                                          start=(k == 0), stop=(k == 7)), R=[hid, d_], W=[pd], sig=(k == 7))
                            op(dve, lambda e: e.scalar_tensor_tensor(
                                out=acc.t[:, ti, fc * 512:(fc + 1) * 512], in0=pd.t[:], scalar=coef.t[:, tix, ei:ei + 1],
                                in1=acc.t[:, ti, fc * 512:(fc + 1) * 512], op0=ALU.mult, op1=ALU.add), R=[pd, coef, acc], W=[acc])
                for ti in range(MB // 128):
                    r0 = m0 + ti * 128
                    load(x2t, x2t.t[:], X2[r0:r0 + 128, :])
                    op(dve, lambda e: e.tensor_tensor(out=acc.t[:, ti, :], in0=acc.t[:, ti, :], in1=g2B.t[:], op=ALU.mult),
                       R=[acc, g2B], W=[acc])
                    op(dve, lambda e: e.tensor_tensor(out=x2t.t[:], in0=x2t.t[:], in1=acc.t[:, ti, :], op=ALU.add),
                       R=[x2t, acc], W=[x2t])
                    with ExitStack() as el:
                        rs = rms_rstd(el, x2t, 128, D, junk)
                        op(dve, lambda e: e.scalar_tensor_tensor(out=x2t.t[:], in0=x2t.t[:], scalar=rs.t[:, 0:1], in1=fnw.t[:],
                                                                 op0=ALU.mult, op1=ALU.mult), R=[x2t, rs, fnw], W=[x2t])
                    store(x2t, out_d[r0:r0 + 128, :], x2t.t[:])
            kb.barrier()
    return nc


def col_layout(v, nchunk):
    return np.ascontiguousarray(np.asarray(v, np.float32).reshape(nchunk, 128).T)


def host_inputs(inp, b, p, SEQ):
    f = lambda a: np.ascontiguousarray(np.asarray(a, np.float32))
    m = {}
    m["x"] = f(inp["x"][b, :SEQ])
    m["ctx"] = f(inp["ctx"][b])
    cv = np.stack([col_layout(inp["c"][b], 16), col_layout(inp["c_ctx"], 16)], axis=-1)
    m["cvec"] = f(cv)
    m["ada_w"] = f(inp["ada_w"][0])
    m["ada_b"] = f(np.broadcast_to(inp["ada_b"][0][None, :], (2, 6 * D)))
    m["n1w"] = col_layout(inp["norm1_w"][0], 16)
    m["n2w"] = col_layout(inp["norm2_w"][0], 16)
    m["fnw"] = f(np.broadcast_to(np.asarray(inp["final_norm_w"])[None, :], (128, D)))
    m["w_in"] = f(inp["w_in"][0])
    cw = np.asarray(inp["ssm_conv_w"][0], np.float32)
    m["cw"] = f(cw.reshape(7, 48, 128).transpose(2, 1, 0))
    m["cb"] = col_layout(inp["ssm_conv_b"][0], 48)
    m["dtb"] = f(np.broadcast_to(np.asarray(inp["dt_bias"][0])[None], (128, 2, 64)))
    m["alog"] = f(np.broadcast_to(np.asarray(inp["a_log"][0])[None], (128, 2, 64)))
    m["dsk"] = f(np.broadcast_to(np.asarray(inp["d_skip"][0])[None], (128, 2, 64)))
    m["snw"] = col_layout(inp["ssm_norm_w"][0], 32)
    m["ssm_out_w"] = f(inp["ssm_out_w"][0])
    cfw = np.asarray(inp["cf_dw_w"][0], np.float32)
    m["cfw"] = f(cfw.reshape(31, 16, 128).transpose(2, 1, 0))
    m["cfb"] = col_layout(inp["cf_dw_b"][0], 16)
    m["lnw"] = col_layout(inp["cf_ln_w"][0], 16)
    m["lnb"] = col_layout(inp["cf_ln_b"][0], 16)
    m["cf_out_w"] = f(inp["cf_out_w"][0])
    m["cfob"] = col_layout(inp["cf_out_b"][0], 16)
    m["w_o"] = f(inp["w_o"][0])
    rw = np.concatenate([np.asarray(inp["router_group_w"][0]), np.asarray(inp["router_expert_w"][0])], axis=1)
    m["rw"] = f(rw.reshape(16, 128, 72).transpose(1, 0, 2))
    rb = np.concatenate([np.asarray(inp["router_group_b"][0]), np.asarray(inp["router_expert_b"][0])])
    m["rb"] = f(np.broadcast_to(rb[None, :], (128, 72)))
    gs = np.zeros((128, 2, 8), np.float32)
    gs[:, 0, 2 * p] = 1.0
    gs[:, 1, 2 * p + 1] = 1.0
    m["gsel"] = gs
    e0 = 16 * p
    m["eg"] = f(inp["expert_w_gate"][0][e0:e0 + EL])
    m["eu"] = f(inp["expert_w_up"][0][e0:e0 + EL])
    m["ed"] = f(inp["expert_w_down"][0][e0:e0 + EL])
    m["ident"] = np.eye(128, dtype=np.float32)
    k = np.arange(128)
    tri = np.zeros((128, 2, 128), np.float32)
    tri[:, 0, :] = (k[:, None] <= k[None, :])
    tri[:, 1, :] = (k[:, None] >= k[None, :])
    m["tri"] = tri
    m["nmask"] = np.where(tri > 0, 0.0, NEG).astype(np.float32)
    return m


def run(inp, SEQ, SBW, MB, cores):
    nc = build(SEQ, SBW, MB)
    in_maps = [host_inputs(inp, i % 2, i // 2, SEQ) for i in cores]
    res = run_bass_kernel_spmd(nc, in_maps, core_ids=list(range(len(cores))))
    return res.results


def kernel(**inputs):
    SEQ = 8192
    results = run(inputs, SEQ, 1024, 512, list(range(8)))
    out = np.zeros((2, SEQ, D), np.float32)
    for i in range(8):
        b = i % 2
        own = results[i]["own"]
        mask = (own.T.reshape(-1) > 0.5)
        out[b][mask] = results[i]["out"][mask]
    return out
```
